# Optimizing a Trainium2 kernel written in Bass

```python
import math
import jax, jax.numpy as jnp
from jax import lax
import numpy as np

D_MODEL = 1024
BATCH = 4
SEQ = 4096
DEPTH = 4

HEAD_DIM = 64
ROPE_THETA = 10000.0
BLOCK_Q = 128
NEG_INF = -1e30
NORM_EPS = 1e-6

DA_HEADS = 4
DA_QK = DA_HEADS * 2 * HEAD_DIM
DA_V = DA_HEADS * 2 * HEAD_DIM

GRID_W = 64
NA_HEADS = 4
NA_WIDTH = NA_HEADS * HEAD_DIM
NA_WIN_ROWS = 8
NA_WIN_COLS = 16

DIL_CONFIGS = ((128, 1), (512, 4), (2048, 16))
DIL_GROUPS = 3
DIL_HEADS = 4
DIL_WIDTH = DIL_GROUPS * DIL_HEADS * HEAD_DIM
DIL_OUT = DIL_HEADS * HEAD_DIM

N_BRANCHES = 3
IN_SIZES = (DA_QK, DA_QK, DA_V, NA_WIDTH, NA_WIDTH, NA_WIDTH,
            DIL_WIDTH, DIL_WIDTH, DIL_WIDTH, N_BRANCHES * D_MODEL)
IN_WIDTH = 2 * DA_QK + DA_V + 3 * NA_WIDTH + 3 * DIL_WIDTH + N_BRANCHES * D_MODEL

N_GROUPS = 4
EXPERTS_PER_GROUP = 8
N_EXPERTS = N_GROUPS * EXPERTS_PER_GROUP
TOP_K_FINE = 2
EXPERT_HIDDEN = 512
MOE_BLOCK = 128

kernel_name = 'hybrid_diff_na_dilated_hmoe_adaln'


def rms_norm(x, g):
    xf = x.astype(jnp.float32)
    y = xf * lax.rsqrt(jnp.mean(xf * xf, axis=-1, keepdims=True) + NORM_EPS)
    return (y * g.astype(jnp.float32)).astype(x.dtype)


def rotary_tables(seq, dim, dtype):
    inv = 1.0 / (ROPE_THETA ** (jnp.arange(0, dim, 2, dtype=jnp.float32) / dim))
    ang = jnp.arange(seq, dtype=jnp.float32)[:, None] * inv[None, :]
    ang = jnp.concatenate([ang, ang], axis=-1)
    return jnp.cos(ang).astype(dtype), jnp.sin(ang).astype(dtype)


def apply_rotary(x, cos, sin):
    x1, x2 = jnp.split(x, 2, axis=-1)
    return x * cos + jnp.concatenate([-x2, x1], axis=-1) * sin


def diff_attention(q, k, v, lam_params, subln_g, lam_init, cos, sin):
    B, S, _ = q.shape
    H, dh = DA_HEADS, HEAD_DIM
    q = apply_rotary(q.reshape(B, S, H, 2, dh).transpose(0, 2, 3, 1, 4), cos, sin)
    k = apply_rotary(k.reshape(B, S, H, 2, dh).transpose(0, 2, 3, 1, 4), cos, sin)
    v = v.reshape(B, S, H, 2 * dh).transpose(0, 2, 1, 3)
    lp = lam_params.astype(jnp.float32)
    lam = jnp.exp(jnp.sum(lp[0] * lp[1])) - jnp.exp(jnp.sum(lp[2] * lp[3])) + lam_init
    nblk = S // BLOCK_Q
    qblocks = q.reshape(B, H, 2, nblk, BLOCK_Q, dh).transpose(3, 0, 1, 2, 4, 5)
    scale = HEAD_DIM ** -0.5

    def block(qi):
        s = jnp.einsum('bhmqd,bhmkd->bhmqk', qi, k).astype(jnp.float32) * scale
        p = jax.nn.softmax(s, axis=-1)
        a = p[:, :, 0] - lam * p[:, :, 1]
        return jnp.einsum('bhqk,bhkd->bhqd', a.astype(v.dtype), v)

    o = lax.map(block, qblocks)
    o = o.transpose(1, 0, 3, 2, 4).reshape(B, S, H, 2 * dh)
    o = rms_norm(o, subln_g) * (1.0 - lam_init)
    return o.reshape(B, S, H * 2 * dh)


def neighbourhood_attention(q, k, v, rpb):
    B, S, _ = q.shape
    H, dh = NA_HEADS, HEAD_DIM
    rows = S // GRID_W
    kh = min(NA_WIN_ROWS, rows)
    kw = NA_WIN_COLS
    L = kh * kw
    r = jnp.arange(rows)
    cq = jnp.arange(GRID_W)
    key_rows = jnp.clip(r - kh // 2, 0, rows - kh)[:, None] + jnp.arange(kh)[None, :]
    key_cols = jnp.clip(cq - kw // 2, 0, GRID_W - kw)[:, None] + jnp.arange(kw)[None, :]
    kidx = (key_rows[:, None, :, None] * GRID_W + key_cols[None, :, None, :]).reshape(rows, GRID_W, L)
    br = key_rows - r[:, None] + (NA_WIN_ROWS - 1)
    bc = key_cols - cq[:, None] + (NA_WIN_COLS - 1)
    bias = rpb[:, br[:, None, :, None], bc[None, :, None, :]]
    bias = bias.reshape(H, rows, GRID_W, L).transpose(1, 0, 2, 3).astype(jnp.float32)
    qr = q.reshape(B, rows, GRID_W, H, dh).transpose(1, 0, 3, 2, 4)
    kt = k.reshape(B, S, H, dh).transpose(0, 2, 1, 3)
    vt = v.reshape(B, S, H, dh).transpose(0, 2, 1, 3)
    scale = HEAD_DIM ** -0.5

    def row_block(args):
        qi, idx, bi = args
        kg = kt[:, :, idx]
        vg = vt[:, :, idx]
        s = jnp.einsum('bhqd,bhqld->bhql', qi, kg).astype(jnp.float32) * scale + bi
        p = jax.nn.softmax(s, axis=-1).astype(vg.dtype)
        return jnp.einsum('bhql,bhqld->bhqd', p, vg)

    o = lax.map(row_block, (qr, kidx, bias))
    return o.transpose(1, 0, 3, 2, 4).reshape(B, S, H * dh)


def dilated_group(q, k, v, window, dilation):
    B, H, S, dh = q.shape
    n_side = window // 2 // dilation
    offs = dilation * jnp.arange(-n_side, n_side + 1)
    nblk = S // BLOCK_Q
    qblocks = q.reshape(B, H, nblk, BLOCK_Q, dh).transpose(2, 0, 1, 3, 4)
    tblocks = jnp.arange(S).reshape(nblk, BLOCK_Q)
    scale = HEAD_DIM ** -0.5

    def block(args):
        qi, t = args
        pos = t[:, None] + offs[None, :]
        valid = (pos >= 0) & (pos < S)
        pos = jnp.clip(pos, 0, S - 1)
        kk = k[:, :, pos]
        vv = v[:, :, pos]
        s = jnp.einsum('bhqd,bhqld->bhql', qi, kk).astype(jnp.float32) * scale
        s = jnp.where(valid, s, NEG_INF)
        lse = jax.nn.logsumexp(s, axis=-1)
        p = jnp.exp(s - lse[..., None])
        return jnp.einsum('bhql,bhqld->bhqd', p.astype(vv.dtype), vv), lse

    o, lse = lax.map(block, (qblocks, tblocks))
    o = o.transpose(1, 2, 0, 3, 4).reshape(B, H, S, dh)
    lse = lse.transpose(1, 2, 0, 3).reshape(B, H, S)
    return o, lse


def dilated_attention(q, k, v, cos, sin):
    B, S, _ = q.shape
    G, H, dh = DIL_GROUPS, DIL_HEADS, HEAD_DIM
    q = apply_rotary(q.reshape(B, S, G, H, dh).transpose(2, 0, 3, 1, 4), cos, sin)
    k = apply_rotary(k.reshape(B, S, G, H, dh).transpose(2, 0, 3, 1, 4), cos, sin)
    v = v.reshape(B, S, G, H, dh).transpose(2, 0, 3, 1, 4)
    outs, lses = [], []
    for g, (window, dilation) in enumerate(DIL_CONFIGS):
        o, lse = dilated_group(q[g], k[g], v[g], window, dilation)
        outs.append(o)
        lses.append(lse)
    alpha = jax.nn.softmax(jnp.stack(lses, axis=0), axis=0)
    o = jnp.sum(alpha[..., None].astype(v.dtype) * jnp.stack(outs, axis=0), axis=0)
    return o.transpose(0, 2, 1, 3).reshape(B, S, H * dh)


def hybrid_mixer(h, w_in, da_lam, da_subln_g, lam_init, na_rpb, w_proj_a, w_proj_n, w_proj_d, w_out, cos, sin):
    z = h @ w_in
    splits = np.cumsum(IN_SIZES)[:-1].tolist()
    q_a, k_a, v_a, q_n, k_n, v_n, q_d, k_d, v_d, gates = jnp.split(z, splits, axis=-1)
    o_a = diff_attention(q_a, k_a, v_a, da_lam, da_subln_g, lam_init, cos, sin)
    o_n = neighbourhood_attention(q_n, k_n, v_n, na_rpb)
    o_d = dilated_attention(q_d, k_d, v_d, cos, sin)
    g_a, g_n, g_d = jnp.split(jax.nn.sigmoid(gates), N_BRANCHES, axis=-1)
    merged = g_a * (o_a @ w_proj_a) + g_n * (o_n @ w_proj_n) + g_d * (o_d @ w_proj_d)
    return merged @ w_out


def grouped_swiglu(xt, expert, weight, w1, w3, w2):
    N, D = xt.shape
    A = N * TOP_K_FINE
    flat_e = expert.reshape(-1)
    flat_w = weight.reshape(-1)
    order = jnp.argsort(flat_e)
    sorted_e = flat_e[order]
    tok = order // TOP_K_FINE
    sizes = jnp.bincount(flat_e, length=N_EXPERTS)
    padded = ((sizes + MOE_BLOCK - 1) // MOE_BLOCK) * MOE_BLOCK
    seg_start = jnp.cumsum(sizes) - sizes
    pad_end = jnp.cumsum(padded)
    pad_start = pad_end - padded
    dest = pad_start[sorted_e] + (jnp.arange(A) - seg_start[sorted_e])
    n_blocks = A // MOE_BLOCK + N_EXPERTS
    M = n_blocks * MOE_BLOCK
    slot_tok = jnp.zeros((M,), jnp.int32).at[dest].set(tok.astype(jnp.int32))
    slot_w = jnp.zeros((M,), jnp.float32).at[dest].set(flat_w[order])
    block_e = jnp.searchsorted(pad_end, jnp.arange(n_blocks) * MOE_BLOCK, side='right')
    block_e = jnp.minimum(block_e, N_EXPERTS - 1)
    xs = xt[slot_tok].reshape(n_blocks, MOE_BLOCK, D)

    def run(args):
        xb, e = args
        return (jax.nn.silu(xb @ w1[e]) * (xb @ w3[e])) @ w2[e]

    y = lax.map(run, (xs, block_e)).reshape(M, D)
    y = y * slot_w[:, None].astype(y.dtype)
    return jnp.zeros_like(xt).at[slot_tok].add(y)


def hier_moe(h, wg, bg, we, be, w1, w3, w2):
    B, S, D = h.shape
    xt = h.reshape(B * S, D)
    N = xt.shape[0]
    pg = jax.nn.softmax((xt @ wg + bg).astype(jnp.float32), axis=-1)
    pg_top, g_idx = lax.top_k(pg, 1)
    le = (xt @ we + be).astype(jnp.float32).reshape(N, N_GROUPS, EXPERTS_PER_GROUP)
    le = jnp.take_along_axis(le, g_idx[:, :, None], axis=1)[:, 0]
    pe_top, e_idx = lax.top_k(jax.nn.softmax(le, axis=-1), TOP_K_FINE)
    pe_top = pe_top / jnp.sum(pe_top, axis=-1, keepdims=True)
    weight = pg_top * pe_top
    expert = g_idx * EXPERTS_PER_GROUP + e_idx
    return grouped_swiglu(xt, expert, weight, w1, w3, w2).reshape(B, S, D)


def setup_inputs(seed: int = 0) -> dict:
    key = jax.random.key(seed)
    ks = jax.random.split(key, 24)
    f32 = jnp.float32
    D = D_MODEL

    def nrm(k, shape, scale):
        return jax.random.normal(k, shape, f32) * scale

    return {
        'x': nrm(ks[0], (BATCH, SEQ, D), 1.0),
        'c': nrm(ks[1], (BATCH, D), 1.0),
        'ada_w': nrm(ks[2], (DEPTH, D, 6 * D), 0.5 * D ** -0.5),
        'ada_b': nrm(ks[3], (DEPTH, 6 * D), 0.02),
        'mix_norm_g': 1.0 + nrm(ks[4], (DEPTH, D), 0.02),
        'ffn_norm_g': 1.0 + nrm(ks[5], (DEPTH, D), 0.02),
        'w_in': nrm(ks[6], (DEPTH, D, IN_WIDTH), D ** -0.5),
        'da_lambda': nrm(ks[7], (DEPTH, 4, HEAD_DIM), 0.1),
        'da_subln_g': 1.0 + nrm(ks[8], (DEPTH, 2 * HEAD_DIM), 0.02),
        'na_rpb': nrm(ks[9], (DEPTH, NA_HEADS, 2 * NA_WIN_ROWS - 1, 2 * NA_WIN_COLS - 1), 0.02),
        'w_proj_a': nrm(ks[10], (DEPTH, DA_V, D), DA_V ** -0.5),
        'w_proj_n': nrm(ks[11], (DEPTH, NA_WIDTH, D), NA_WIDTH ** -0.5),
        'w_proj_d': nrm(ks[12], (DEPTH, DIL_OUT, D), DIL_OUT ** -0.5),
        'w_out': nrm(ks[13], (DEPTH, D, D), D ** -0.5),
        'router_group_w': nrm(ks[14], (DEPTH, D, N_GROUPS), D ** -0.5),
        'router_group_b': nrm(ks[15], (DEPTH, N_GROUPS), 0.01),
        'router_expert_w': nrm(ks[16], (DEPTH, D, N_EXPERTS), D ** -0.5),
        'router_expert_b': nrm(ks[17], (DEPTH, N_EXPERTS), 0.01),
        'expert_w1': nrm(ks[18], (DEPTH, N_EXPERTS, D, EXPERT_HIDDEN), D ** -0.5),
        'expert_w3': nrm(ks[19], (DEPTH, N_EXPERTS, D, EXPERT_HIDDEN), D ** -0.5),
        'expert_w2': nrm(ks[20], (DEPTH, N_EXPERTS, EXPERT_HIDDEN, D), EXPERT_HIDDEN ** -0.5),
        'final_norm_g': 1.0 + nrm(ks[21], (D,), 0.02),
    }


def reference(x, c, ada_w, ada_b, mix_norm_g, ffn_norm_g, w_in, da_lambda, da_subln_g, na_rpb,
              w_proj_a, w_proj_n, w_proj_d, w_out, router_group_w, router_group_b,
              router_expert_w, router_expert_b, expert_w1, expert_w3, expert_w2, final_norm_g):
    B, S, D = x.shape
    cos, sin = rotary_tables(S, HEAD_DIM, x.dtype)
    c_act = jax.nn.silu(c)
    for l in range(DEPTH):
        mod = c_act @ ada_w[l] + ada_b[l]
        shift_m, scale_m, gate_m, shift_f, scale_f, gate_f = jnp.split(mod[:, None, :], 6, axis=-1)
        lam_init = 0.8 - 0.6 * math.exp(-0.3 * l)
        h = rms_norm(x, mix_norm_g[l]) * (1.0 + scale_m) + shift_m
        y = hybrid_mixer(h, w_in[l], da_lambda[l], da_subln_g[l], lam_init, na_rpb[l],
                         w_proj_a[l], w_proj_n[l], w_proj_d[l], w_out[l], cos, sin)
        x = x + gate_m * y
        h = rms_norm(x, ffn_norm_g[l]) * (1.0 + scale_f) + shift_f
        y = hier_moe(h, router_group_w[l], router_group_b[l], router_expert_w[l], router_expert_b[l],
                     expert_w1[l], expert_w3[l], expert_w2[l])
        x = x + gate_f * y
    return rms_norm(x, final_norm_g)
```

```python
from concourse.bass_utils import run_bass_kernel_spmd
import numpy as np
import concourse.bass as bass
import concourse.mybir as mybir
from contextlib import ExitStack

F32, BF16, I32 = mybir.dt.float32, mybir.dt.bfloat16, mybir.dt.int32
AF = mybir.ActivationFunctionType
ALU = mybir.AluOpType
AX = mybir.AxisListType

EPOCH_MAX = 16000
N_DMA_SEMS = 20


class Buf:
    __slots__ = ("name", "w", "r", "rd")

    def __init__(self, name):
        self.name = name
        self.w = None
        self.r = {}
        self.rd = []


class _Op:
    __slots__ = ("eng", "is_dma", "sem", "val")

    def __init__(self, eng, is_dma):
        self.eng = eng
        self.is_dma = is_dma
        self.sem = None
        self.val = None


class FW:
    def __init__(self, nc, marks=None):
        self.nc = nc
        self.dry = marks is None
        self.marks = set() if marks is None else marks
        self.ops = []
        self.streams = {"pe": nc.tensor, "act": nc.scalar, "dve": nc.vector,
                        "pool": nc.gpsimd, "sp": nc.sync}
        self.es = ExitStack()
        self.csems = {e: [] for e in ("pe", "act", "dve", "pool")}
        self.ccount = {e: 0 for e in ("pe", "act", "dve", "pool")}
        self.dsems = []
        self.dsem_val = []
        self.dsem_last = []
        self.ndma = 0
        self.known = {s: {} for s in self.streams}
        self.last_c = {}
        self.open_dmas = []
        self.bar = {s: None for s in self.streams}
        self.n_wait = 0

    def buf(self, name="b"):
        return Buf(name)

    def bufs(self, name, n):
        return [Buf(f"{name}{i}") for i in range(n)]

    def _collect(self, opid, eng, is_dma, reads, writes):
        dc = {}
        dd = set()
        ops = self.ops

        def add(o, raw):
            if o is None:
                return
            oo = ops[o]
            if oo.is_dma:
                dd.add(o)
                return
            if not is_dma and oo.eng == eng:
                if not raw or eng == "pe":
                    return
            if dc.get(oo.eng, -1) < o:
                dc[oo.eng] = o

        wset = set(id(b) for b in writes)
        for b in reads:
            add(b.w, True)
        for b in writes:
            add(b.w, False)
            for o in b.r.values():
                add(o, False)
            for o in b.rd:
                add(o, False)
        for b in writes:
            b.w = opid
            b.r = {}
            b.rd = []
        for b in reads:
            if id(b) in wset:
                continue
            if is_dma:
                b.rd.append(opid)
            else:
                b.r[eng] = opid
        return dc, dd

    def barrier(self):
        snap = (dict(self.last_c), list(self.open_dmas))
        self.open_dmas = []
        for s in self.bar:
            self.bar[s] = snap

    def _sem_for(self, eng, n):
        ep = (n - 1) // EPOCH_MAX
        while len(self.csems[eng]) <= ep:
            self.csems[eng].append(
                self.es.enter_context(self.nc.semaphore(f"c_{eng}_{len(self.csems[eng])}")))
        return self.csems[eng][ep], (n - 1) % EPOCH_MAX + 1, ep

    def _wait(self, stream, sem, key, val):
        kn = self.known[stream]
        if kn.get(key, 0) >= val:
            return
        kn[key] = val
        self.n_wait += 1
        self.streams[stream].wait_ge(sem, val)

    def _do(self, stream, eng, is_dma, fn, reads, writes):
        opid = len(self.ops)
        op = _Op(eng, is_dma)
        self.ops.append(op)
        dc, dd = self._collect(opid, eng, is_dma, reads, writes)
        b = self.bar[stream]
        if b is not None:
            self.bar[stream] = None
            for e, o in b[0].items():
                if is_dma or e != eng:
                    if dc.get(e, -1) < o:
                        dc[e] = o
            dd.update(b[1])
        if self.dry:
            for o in dc.values():
                self.marks.add(o)
            if is_dma:
                self.open_dmas.append(opid)
            else:
                self.last_c[eng] = opid
            return
        for e, o in dc.items():
            oo = self.ops[o]
            assert oo.sem is not None, (e, o)
            self._wait(stream, oo.sem[0], ("c", e, oo.sem[1]), oo.val)
        for o in dd:
            oo = self.ops[o]
            self._wait(stream, oo.sem[0], ("d", oo.sem[1]), oo.val)
        if is_dma:
            k = self.ndma % N_DMA_SEMS
            self.ndma += 1
            if len(self.dsems) <= k:
                self.dsems.append(self.es.enter_context(self.nc.semaphore(f"d_{k}")))
                self.dsem_val.append(0)
            if self.dsem_val[k] > 0:
                self._wait(stream, self.dsems[k], ("d", k), self.dsem_val[k])
            inst = fn(self.streams[stream])
            self.dsem_val[k] += 16
            inst.then_inc(self.dsems[k], 16)
            op.sem = (self.dsems[k], k)
            op.val = self.dsem_val[k]
            self.open_dmas.append(opid)
        else:
            inst = fn(self.streams[stream])
            if opid in self.marks:
                self.ccount[eng] += 1
                sem, val, ep = self._sem_for(eng, self.ccount[eng])
                inst.then_inc(sem, 1)
                op.sem = (sem, ep)
                op.val = val
            self.last_c[eng] = opid

    def op(self, eng, fn, reads=(), writes=()):
        self._do(eng, eng, False, fn, reads, writes)

    def dma(self, queue, out, in_, reads=(), writes=(), **kw):
        self._do(queue, "dma", True,
                 lambda s: s.dma_start(out=out, in_=in_, **kw), reads, writes)

    def finish(self):
        self.barrier()
        if self.dry:
            for o in self.last_c.values():
                self.marks.add(o)
            return
        snap = self.bar["sp"]
        for e, o in snap[0].items():
            oo = self.ops[o]
            self._wait("sp", oo.sem[0], ("c", e, oo.sem[1]), oo.val)
        for o in snap[1]:
            oo = self.ops[o]
            self._wait("sp", oo.sem[0], ("d", oo.sem[1]), oo.val)
        for k, s in enumerate(self.dsems):
            self._wait("sp", s, ("d", k), self.dsem_val[k])


D = 1024
S = 4096
TOWN = 2048
NCTX = 4096
OWN0 = 1024
INW = 7680
EPS = 1e-6
NEXP = 32
HID = 512

WBLOCKS = [
    (0, 512, "qr", 0), (512, 512, "kr", 0), (1024, 512, "v", 0),
    (1536, 256, "q", 4), (1792, 256, "k", 4), (2048, 256, "v", 512),
    (2304, 512, "qr", 6), (2816, 256, "qr", 10),
    (3072, 512, "kr", 6), (3584, 256, "kr", 10),
    (3840, 512, "v", 768), (4352, 256, "v", 1280),
] + [(4608 + 512 * j, 512, "g", 4 * j) for j in range(6)]


class Prog:
    def __init__(self, nc, fw, debug=False, stop_after=None, n_exp=NEXP):
        self.nc, self.fw, self.debug, self.stop_after = nc, fw, debug, stop_after
        self.n_exp = n_exp
        self.t = {}

    def dram_in(self, name, shape, dt=F32):
        self.t[name] = self.nc.dram_tensor(name, list(shape), dt, kind="ExternalInput").ap()
        return self.t[name]

    def dram_out(self, name, shape, dt=F32):
        self.t[name] = self.nc.dram_tensor(name, list(shape), dt, kind="ExternalOutput").ap()
        return self.t[name]

    def dram_scr(self, name, shape, dt):
        if self.debug:
            return self.dram_out(name, shape, dt)
        self.t[name] = self.nc.dram_tensor(name, list(shape), dt).ap()
        return self.t[name]

    def declare(self):
        di = self.dram_in
        di("xc", [NCTX, D]); di("c_col", [128, 8]); di("ada_w", [D, 6 * D]); di("ada_b", [1, 6 * D])
        di("ada_b_col", [128, 48]); di("g_mix_col", [128, 8]); di("g_ffn_col", [128, 8])
        di("w_in", [D, INW]); di("da_lambda", [1, 256]); di("subln_col", [128, 1]); di("lam_init", [1, 2])
        di("na_tab", [4, 15, 64, 64]); di("na_rowmask", [24, 128, 512]); di("dil_mask", [7, 128, 128])
        di("w_proj_a", [512, D]); di("w_proj_n", [256, D]); di("w_proj_d", [256, D]); di("w_out", [D, D])
        di("w_router", [D, 36]); di("b_router", [1, 36])
        di("w1", [self.n_exp, D, HID]); di("w3", [self.n_exp, D, HID]); di("w2", [self.n_exp, HID, D])
        di("g_fin", [1, D])
        di("cosq", [128, TOWN]); di("sinq", [128, TOWN]); di("cosk", [128, NCTX]); di("sink", [128, NCTX])
        di("ident", [128, 128]); di("rotm", [128, 128])
        self.dram_out("x_out", [TOWN, D]); self.dram_out("xn_out", [TOWN, D])
        ds = self.dram_scr
        ds("qT_d", [12, 128, TOWN], BF16); ds("kT_d", [12, 128, NCTX], BF16)
        ds("v_d", [NCTX, 1536], BF16); ds("gT_d", [24, 128, TOWN], BF16)
        ds("oT_d", [8, 128, TOWN], BF16); ds("x_mid", [TOWN, D], F32)
        if self.debug:
            self.dram_out("dbg_mod", [128, 2 * D + 32]); self.dram_out("dbg_hT", [128, 8, NCTX], BF16); self.dram_out("dbg_Wt", [TOWN, 32])

    def sb(self, es, name, shape, dt):
        self._uid = getattr(self, "_uid", 0) + 1
        return es.enter_context(self.nc.sbuf_tensor(f"{name}_{self._uid}", list(shape), dt))

    def ps(self, es, name, shape, dt=F32):
        self._uid = getattr(self, "_uid", 0) + 1
        return es.enter_context(self.nc.psum_tensor(f"{name}_{self._uid}", list(shape), dt))

    def build(self):
        nc, fw, t = self.nc, self.fw, self.t
        self.declare()
        with ExitStack() as top:
            self.top = top
            self.consts(top)
            self.phase_mod()
            fw.barrier()
            with ExitStack() as hs:
                self.hT = self.sb(hs, "hT", [128, 8, NCTX], BF16)
                self.b_hT = fw.bufs("hT", NCTX // 128)
                self.phase_norm(t["xc"], NCTX // 128, self.gs_m, self.sh_m, router=False)
                if self.stop_after == "norm":
                    return self.end()
                self.phase_win()
                fw.barrier()
            if self.stop_after == "win":
                return self.end()
            self.phase_da()
            if self.stop_after == "da":
                return self.end()
            self.phase_na()
            if self.stop_after == "na":
                return self.end()
            self.phase_dil()
            if self.stop_after == "dil":
                return self.end()
            self.phase_proj()
            if self.stop_after == "proj":
                return self.end()
            self.phase_ffn()
            return self.end()

    def end(self):
        self.fw.finish()

    def consts(self, es):
        nc, fw, t = self.nc, self.fw, self.t
        self.ident32 = self.sb(es, "ident32", [128, 128], F32)
        self.identb = self.sb(es, "identb", [128, 128], BF16)
        self.rotm = self.sb(es, "rotm_sb", [128, 128], BF16)
        self.onesb = self.sb(es, "onesb", [128, 128], BF16)
        self.ones32 = self.sb(es, "ones32", [128, 128], F32)
        self.modcol = self.sb(es, "modcol", [128, 32], F32)
        self.gs_m = self.sb(es, "gs_m", [128, 8], F32)
        self.gs_f = self.sb(es, "gs_f", [128, 8], F32)
        self.gate_bc = self.sb(es, "gate_bc", [128, 2 * D], F32)
        self.eps_col = self.sb(es, "eps_col", [128, 1], F32)
        self.b_const = fw.buf("consts")
        self.b_mod = fw.buf("mod")
        fw.dma("sp", self.ident32[:], t["ident"][:, :], writes=[self.b_const])
        fw.dma("pool", self.identb[:], t["ident"][:, :], writes=[self.b_const])
        fw.dma("pool", self.rotm[:], t["rotm"][:, :], writes=[self.b_const])
        fw.op("dve", lambda e: e.memset(self.onesb[:], 1.0), writes=[self.b_const])
        fw.op("dve", lambda e: e.memset(self.ones32[:], 1.0), writes=[self.b_const])
        fw.op("dve", lambda e: e.memset(self.eps_col[:], EPS), writes=[self.b_const])

    def phase_mod(self):
        nc, fw, t = self.nc, self.fw, self.t
        with ExitStack() as es:
            c_col = self.sb(es, "c_col_sb", [128, 8], F32)
            c_act = self.sb(es, "c_act", [128, 8], F32)
            c_rep = self.sb(es, "c_rep", [128, 8, 128], F32)
            adab_row = self.sb(es, "adab_row", [1, 6 * D], F32)
            adab_col = self.sb(es, "adab_col", [128, 48], F32)
            gm = self.sb(es, "gm_col", [128, 8], F32)
            gf = self.sb(es, "gf_col", [128, 8], F32)
            wblk = [self.sb(es, f"adaw{i}", [128, 8, 512], F32) for i in range(2)]
            ps_bc = [self.ps(es, f"ps_bc{i}", [128, 512]) for i in range(2)]
            ps_col = [self.ps(es, f"ps_col{i}", [128, 4]) for i in range(2)]
            b_c = fw.buf("c"); b_w = fw.bufs("adaw", 2); b_pb = fw.bufs("psbc", 2); b_pc = fw.bufs("pscol", 2)
            fw.dma("sp", c_col[:], t["c_col"][:, :], writes=[b_c])
            fw.dma("sp", adab_row[:], t["ada_b"][:, :], writes=[b_c])
            fw.dma("sp", adab_col[:], t["ada_b_col"][:, :], writes=[b_c])
            fw.dma("sp", gm[:], t["g_mix_col"][:, :], writes=[b_c])
            fw.dma("sp", gf[:], t["g_ffn_col"][:, :], writes=[b_c])
            b_ca = fw.buf("c_act")
            fw.op("act", lambda e: e.activation(out=c_act[:], in_=c_col[:], func=AF.Silu), reads=[b_c], writes=[b_ca])
            b_cr = fw.buf("c_rep")
            for k in range(8):
                fw.op("dve", lambda e, k=k: e.tensor_scalar(out=c_rep[:, k, :], in0=self.ones32[:], scalar1=c_act[:, k:k + 1],
                                                        scalar2=None, op0=ALU.mult),
                      reads=[b_ca, self.b_const], writes=[b_cr])
            adaw = t["ada_w"].rearrange("(k p) n -> p k n", p=128)
            for j in range(12):
                s = j % 2
                fw.dma("sp", wblk[s][:], adaw[:, :, 512 * j:512 * (j + 1)], writes=[b_w[s]])
                kind = "bc" if j in (4, 5, 10, 11) else "col"
                if kind == "bc":
                    for k in range(8):
                        fw.op("pe", lambda e, k=k, s=s: e.matmul(ps_bc[s][:], c_rep[:, k, :], wblk[s][:, k, :], start=(k == 0), stop=False),
                              reads=[b_cr, b_w[s]], writes=[b_pb[s]])
                    fw.op("pe", lambda e, s=s, j=j: e.matmul(ps_bc[s][:], self.ones32[0:1, :], adab_row[0:1, 512 * j:512 * (j + 1)], start=False, stop=True),
                          reads=[b_c, self.b_const], writes=[b_pb[s]])
                    off = (j - 4) * 512 if j < 6 else D + (j - 10) * 512
                    fw.op("act", lambda e, s=s, off=off: e.activation(out=self.gate_bc[:, off:off + 512], in_=ps_bc[s][:], func=AF.Identity),
                          reads=[b_pb[s]], writes=[self.b_mod])
                else:
                    for cc in range(4):
                        for k in range(8):
                            fw.op("pe", lambda e, k=k, s=s, cc=cc: e.matmul(ps_col[s][:, cc:cc + 1], wblk[s][:, k, 128 * cc:128 * (cc + 1)], c_act[:, k:k + 1],
                                                                         start=(k == 0), stop=(k == 7)),
                                  reads=[b_ca, b_w[s]], writes=[b_pc[s]])
                    jj = {0: 0, 1: 1, 2: 2, 3: 3, 6: 4, 7: 5, 8: 6, 9: 7}[j]
                    fw.op("dve", lambda e, s=s, jj=jj, j=j: e.tensor_tensor(out=self.modcol[:, 4 * jj:4 * jj + 4], in0=ps_col[s][:, :],
                                                                     in1=adab_col[:, 4 * j:4 * j + 4], op=ALU.add),
                          reads=[b_pc[s], b_c], writes=[self.b_mod])
            for (gs, g, so) in ((self.gs_m, gm, 8), (self.gs_f, gf, 24)):
                fw.op("dve", lambda e, gs=gs, g=g, so=so: e.scalar_tensor_tensor(out=gs[:], in0=self.modcol[:, so:so + 8], scalar=1.0, in1=g[:],
                                                                         op0=ALU.add, op1=ALU.mult),
                      reads=[self.b_mod, b_c], writes=[self.b_mod])
            self.sh_m = self.modcol[:, 0:8]
            self.sh_f = self.modcol[:, 16:24]
            if self.debug:
                fw.dma("sp", t["dbg_mod"][:, 0:2 * D], self.gate_bc[:], reads=[self.b_mod])
                fw.dma("sp", t["dbg_mod"][:, 2 * D:2 * D + 32], self.modcol[:], reads=[self.b_mod])
            fw.barrier()

    def phase_norm(self, x_dram, n_tiles, gs_col, sh_col, router):
        nc, fw, t = self.nc, self.fw, self.t
        with ExitStack() as es:
            NB = 3
            xt = [self.sb(es, f"xt{i}", [128, D], F32) for i in range(NB)]
            junk = self.sb(es, "junk", [128, D], BF16)
            xn = [self.sb(es, f"xn{i}", [128, D], F32) for i in range(2)]
            st = [self.sb(es, f"st{i}", [128, 4], F32) for i in range(2)]
            h32 = [self.sb(es, f"h32_{i}", [128, 8, 128], F32) for i in range(2)]
            pT = [self.ps(es, f"pT{i}", [128, 8, 128]) for i in range(2)]
            b_xt = fw.bufs("xt", NB); b_junk = fw.buf("junk"); b_xn = fw.bufs("xn", 2); b_st = fw.bufs("st", 2)
            b_h32 = fw.bufs("h32", 2); b_pT = fw.bufs("pT", 2)
            for i in range(n_tiles):
                a, b2 = i % NB, i % 2
                fw.dma("sp", xt[a][:], x_dram[128 * i:128 * (i + 1), :], writes=[b_xt[a]])
                fw.op("act", lambda e, a=a, b2=b2: e.activation(out=junk[:], in_=xt[a][:], func=AF.Square, accum_out=st[b2][:, 0:1]),
                      reads=[b_xt[a]], writes=[b_junk, b_st[b2]])
                fw.op("act", lambda e, b2=b2: e.activation(out=st[b2][:, 1:2], in_=st[b2][:, 0:1], func=AF.Sqrt, scale=1.0 / D, bias=self.eps_col[:, 0:1]),
                      reads=[b_st[b2], self.b_const], writes=[b_st[b2]])
                fw.op("dve", lambda e, b2=b2: e.reciprocal(out=st[b2][:, 2:3], in_=st[b2][:, 1:2]), reads=[b_st[b2]], writes=[b_st[b2]])
                fw.op("dve", lambda e, a=a, b2=b2: e.tensor_scalar(out=xn[b2][:], in0=xt[a][:], scalar1=st[b2][:, 2:3], scalar2=None, op0=ALU.mult),
                      reads=[b_xt[a], b_st[b2]], writes=[b_xn[b2]])
                for k in range(8):
                    fw.op("pe", lambda e, k=k, b2=b2: e.transpose(out=pT[b2][:, k, :], in_=xn[b2][:, 128 * k:128 * (k + 1)], identity=self.ident32[:]),
                          reads=[b_xn[b2], self.b_const], writes=[b_pT[b2]])
                for k in range(8):
                    fw.op("dve", lambda e, k=k, b2=b2: e.tensor_scalar(out=h32[b2][:, k, :], in0=pT[b2][:, k, :], scalar1=gs_col[:, k:k + 1],
                                                                 scalar2=sh_col[:, k:k + 1], op0=ALU.mult, op1=ALU.add),
                          reads=[b_pT[b2], self.b_mod], writes=[b_h32[b2]])
                fw.op("pool", lambda e, b2=b2, i=i: e.tensor_copy(out=self.hT[:, :, 128 * i:128 * (i + 1)], in_=h32[b2][:]),
                      reads=[b_h32[b2]], writes=[self.b_hT[i]])
                if router:
                    self.router_tile(i, h32[b2], b_h32[b2])
            if self.debug and not router:
                fw.dma("sp", t["dbg_hT"][:, :, :], self.hT[:], reads=self.b_hT)
            fw.barrier()

    def phase_win(self):
        nc, fw, t = self.nc, self.fw, self.t
        with ExitStack() as es:
            wb = [self.sb(es, f"wb{i}", [128, 8, 512], BF16) for i in range(2)]
            cosq = self.sb(es, "cosq_sb", [128, TOWN], F32); sinq = self.sb(es, "sinq_sb", [128, TOWN], F32)
            cosk = self.sb(es, "cosk_sb", [128, NCTX], F32); sink = self.sb(es, "sink_sb", [128, NCTX], F32)
            NZ = 3
            zps = [self.ps(es, f"zps{i}", [128, 512]) for i in range(NZ)]
            rps = [self.ps(es, f"rps{i}", [128, 512]) for i in range(2)]
            zsb = [self.sb(es, f"zsb{i}", [128, 512], BF16) for i in range(2)]
            t1 = [self.sb(es, f"t1_{i}", [128, 512], F32) for i in range(2)]
            t2 = [self.sb(es, f"t2_{i}", [128, 512], F32) for i in range(2)]
            NO = 4
            ob = [self.sb(es, f"ob{i}", [128, 512], BF16) for i in range(NO)]
            b_wb = fw.bufs("wb", 2); b_tab = fw.buf("tab"); b_z = fw.bufs("zps", NZ); b_r = fw.bufs("rps", 2)
            b_zsb = fw.bufs("zsb", 2); b_t1 = fw.bufs("t1", 2); b_t2 = fw.bufs("t2", 2); b_ob = fw.bufs("ob", NO)
            self.b_qT = fw.buf("qT_d"); self.b_kT = fw.buf("kT_d"); self.b_v = fw.buf("v_d"); self.b_gT = fw.buf("gT_d")
            fw.dma("sp", cosq[:], t["cosq"][:, :], writes=[b_tab]); fw.dma("sp", sinq[:], t["sinq"][:, :], writes=[b_tab])
            fw.dma("sp", cosk[:], t["cosk"][:, :], writes=[b_tab]); fw.dma("sp", sink[:], t["sink"][:, :], writes=[b_tab])
            win = t["w_in"].rearrange("(k p) n -> p k n", p=128)
            cnt = {"z": 0, "r": 0, "o": 0}

            def load(j):
                c0, ncol, kind, dst = WBLOCKS[j]
                fw.dma("pool", wb[j % 2][:, :, 0:ncol], win[:, :, c0:c0 + ncol], writes=[b_wb[j % 2]])

            load(0)
            for j, (c0, ncol, kind, dst) in enumerate(WBLOCKS):
                if j + 1 < len(WBLOCKS):
                    load(j + 1)
                w = wb[j % 2]; bw = b_wb[j % 2]
                if kind == "v":
                    for tt in range(NCTX // 128):
                        zi = cnt["z"] % NZ; cnt["z"] += 1
                        oi = cnt["o"] % NO; cnt["o"] += 1
                        for k in range(8):
                            fw.op("pe", lambda e, k=k, zi=zi, tt=tt, w=w, ncol=ncol: e.matmul(zps[zi][:, 0:ncol], self.hT[:, k, 128 * tt:128 * (tt + 1)], w[:, k, 0:ncol],
                                                                                 start=(k == 0), stop=(k == 7)),
                                  reads=[self.b_hT[tt], bw], writes=[b_z[zi]])
                        if tt % 2 == 0:
                            fw.op("act", lambda e, zi=zi, oi=oi, ncol=ncol: e.activation(out=ob[oi][:, 0:ncol], in_=zps[zi][:, 0:ncol], func=AF.Copy),
                                  reads=[b_z[zi]], writes=[b_ob[oi]])
                        else:
                            fw.op("dve", lambda e, zi=zi, oi=oi, ncol=ncol: e.tensor_copy(out=ob[oi][:, 0:ncol], in_=zps[zi][:, 0:ncol]),
                                  reads=[b_z[zi]], writes=[b_ob[oi]])
                        fw.dma("sp", t["v_d"][128 * tt:128 * (tt + 1), dst:dst + ncol], ob[oi][:, 0:ncol], reads=[b_ob[oi]], writes=[self.b_v])
                    continue
                own = kind in ("q", "qr", "g")
                ntc = 4 if own else 8
                for cc in range(ncol // 128):
                    for tc in range(ntc):
                        tok0 = (OWN0 if own else 0) + 512 * tc
                        zi = cnt["z"] % NZ; cnt["z"] += 1
                        oi = cnt["o"] % NO; cnt["o"] += 1
                        for k in range(8):
                            fw.op("pe", lambda e, k=k, zi=zi, tok0=tok0, w=w, cc=cc: e.matmul(zps[zi][:], w[:, k, 128 * cc:128 * (cc + 1)], self.hT[:, k, tok0:tok0 + 512],
                                                                                  start=(k == 0), stop=(k == 7)),
                                  reads=[self.b_hT[tok0 // 128 + q] for q in range(4)] + [bw], writes=[b_z[zi]])
                        if kind == "g":
                            fw.op("act", lambda e, zi=zi, oi=oi: e.activation(out=ob[oi][:], in_=zps[zi][:], func=AF.Sigmoid), reads=[b_z[zi]], writes=[b_ob[oi]])
                            dstap = t["gT_d"][dst + cc, :, 512 * tc:512 * (tc + 1)]; bd = self.b_gT
                        elif kind == "q":
                            fw.op("act", lambda e, zi=zi, oi=oi: e.activation(out=ob[oi][:], in_=zps[zi][:], func=AF.Copy, scale=0.125), reads=[b_z[zi]], writes=[b_ob[oi]])
                            dstap = t["qT_d"][dst + cc, :, 512 * tc:512 * (tc + 1)]; bd = self.b_qT
                        elif kind == "k":
                            fw.op("dve", lambda e, zi=zi, oi=oi: e.tensor_copy(out=ob[oi][:], in_=zps[zi][:]), reads=[b_z[zi]], writes=[b_ob[oi]])
                            dstap = t["kT_d"][dst + cc, :, 512 * tc:512 * (tc + 1)]; bd = self.b_kT
                        else:
                            ri = cnt["r"] % 2; cnt["r"] += 1
                            if kind == "qr":
                                ct, stb = cosq[:, 512 * tc:512 * (tc + 1)], sinq[:, 512 * tc:512 * (tc + 1)]
                                dstap = t["qT_d"][dst + cc, :, 512 * tc:512 * (tc + 1)]; bd = self.b_qT
                            else:
                                ct, stb = cosk[:, 512 * tc:512 * (tc + 1)], sink[:, 512 * tc:512 * (tc + 1)]
                                dstap = t["kT_d"][dst + cc, :, 512 * tc:512 * (tc + 1)]; bd = self.b_kT
                            fw.op("act", lambda e, zi=zi, ri=ri: e.activation(out=zsb[ri][:], in_=zps[zi][:], func=AF.Copy), reads=[b_z[zi]], writes=[b_zsb[ri]])
                            fw.op("pe", lambda e, ri=ri: e.matmul(rps[ri][:], self.rotm[:], zsb[ri][:], start=True, stop=True),
                                  reads=[b_zsb[ri], self.b_const], writes=[b_r[ri]])
                            fw.op("pool", lambda e, ri=ri, ct=ct: e.tensor_tensor(out=t1[ri][:], in0=zsb[ri][:], in1=ct, op=ALU.mult),
                                  reads=[b_zsb[ri], b_tab], writes=[b_t1[ri]])
                            fw.op("dve", lambda e, ri=ri, stb=stb: e.tensor_tensor(out=t2[ri][:], in0=rps[ri][:], in1=stb, op=ALU.mult),
                                  reads=[b_r[ri], b_tab], writes=[b_t2[ri]])
                            fw.op("pool", lambda e, ri=ri, oi=oi: e.tensor_tensor(out=ob[oi][:], in0=t1[ri][:], in1=t2[ri][:], op=ALU.add),
                                  reads=[b_t1[ri], b_t2[ri]], writes=[b_ob[oi]])
                        fw.dma("sp", dstap, ob[oi][:], reads=[b_ob[oi]], writes=[bd])
            fw.barrier()

    def phase_da(self):
        nc, fw, t = self.nc, self.fw, self.t
        self.b_oT = fw.bufs("oT_d", 8)
        with ExitStack() as es:
            kT = [self.sb(es, f"da_kT{i}", [128, NCTX], BF16) for i in range(2)]
            qT = [self.sb(es, f"da_qT{i}", [128, TOWN], BF16) for i in range(2)]
            V = [self.sb(es, f"da_V{i}", [128, 32, 128], BF16) for i in range(2)]
            dl = self.sb(es, "da_dl", [1, 256], F32); li = self.sb(es, "da_li", [1, 2], F32)
            tmp = self.sb(es, "da_tmp", [1, 128], F32); sv = self.sb(es, "da_sv", [1, 4], F32)
            lamcol = self.sb(es, "da_lamcol", [128, 2], F32); gcol = self.sb(es, "da_gcol", [128, 1], F32)
            sub = self.sb(es, "da_sub", [128, 1], F32)
            P = [[self.sb(es, f"da_P{i}{m}", [128, 512], BF16) for m in range(2)] for i in range(2)]
            r0 = self.sb(es, "da_r0", [128, 512], F32); r1 = self.sb(es, "da_r1", [128, 512], F32)
            o0 = self.sb(es, "da_o0", [128, 512], F32); o1 = self.sb(es, "da_o1", [128, 512], F32)
            sq = self.sb(es, "da_sq", [128, 512], BF16); rinv = self.sb(es, "da_rinv", [128, 512], F32)
            ob = [self.sb(es, f"da_ob{i}", [128, 512], BF16) for i in range(2)]
            Sp = [[self.ps(es, f"da_S{i}{m}", [128, 512]) for m in range(2)] for i in range(2)]
            Op = [self.ps(es, f"da_O{m}", [128, 512]) for m in range(2)]
            Dp = [self.ps(es, f"da_D{m}", [128, 512]) for m in range(2)]
            b_k = fw.bufs("dak", 2); b_q = fw.bufs("daq", 2); b_V = fw.bufs("daV", 2)
            b_l = fw.buf("dal"); b_lc = fw.buf("dalc")
            b_P = [fw.bufs(f"daP{i}", 2) for i in range(2)]; b_S = [fw.bufs(f"daS{i}", 2) for i in range(2)]
            b_O = fw.bufs("daO", 2); b_D = fw.bufs("daD", 2)
            b_f = fw.buf("dafin"); b_ob = fw.bufs("daob", 2)
            fw.dma("sp", dl[:], t["da_lambda"][:, :], writes=[b_l]); fw.dma("sp", li[:], t["lam_init"][:, :], writes=[b_l])
            fw.dma("sp", sub[:], t["subln_col"][:, :], writes=[b_l])
            fw.op("dve", lambda e: e.tensor_tensor(out=tmp[:, 0:64], in0=dl[:, 0:64], in1=dl[:, 64:128], op=ALU.mult), reads=[b_l], writes=[b_lc])
            fw.op("dve", lambda e: e.tensor_tensor(out=tmp[:, 64:128], in0=dl[:, 128:192], in1=dl[:, 192:256], op=ALU.mult), reads=[b_l], writes=[b_lc])
            fw.op("dve", lambda e: e.reduce_sum(out=sv[:, 0:1], in_=tmp[:, 0:64], axis=AX.X), reads=[b_lc], writes=[b_lc])
            fw.op("dve", lambda e: e.reduce_sum(out=sv[:, 1:2], in_=tmp[:, 64:128], axis=AX.X), reads=[b_lc], writes=[b_lc])
            fw.op("act", lambda e: e.activation(out=sv[:, 2:4], in_=sv[:, 0:2], func=AF.Exp), reads=[b_lc], writes=[b_lc])
            fw.op("dve", lambda e: e.tensor_tensor(out=sv[:, 0:1], in0=sv[:, 3:4], in1=sv[:, 2:3], op=ALU.subtract), reads=[b_lc], writes=[b_lc])
            fw.op("dve", lambda e: e.tensor_tensor(out=sv[:, 0:1], in0=sv[:, 0:1], in1=li[:, 0:1], op=ALU.subtract), reads=[b_lc, b_l], writes=[b_lc])
            fw.op("dve", lambda e: e.tensor_copy(out=sv[:, 1:2], in_=li[:, 1:2]), reads=[b_lc, b_l], writes=[b_lc])
            fw.op("pe", lambda e: e.matmul(Sp[0][0][:, 0:2], self.ones32[0:1, :], sv[0:1, 0:2], start=True, stop=True), reads=[b_lc, self.b_const], writes=[b_S[0][0]])
            fw.op("dve", lambda e: e.tensor_copy(out=lamcol[:], in_=Sp[0][0][:, 0:2]), reads=[b_S[0][0]], writes=[b_lc])
            fw.op("dve", lambda e: e.tensor_tensor(out=gcol[:], in0=sub[:], in1=lamcol[:, 1:2], op=ALU.mult), reads=[b_lc, b_l], writes=[b_lc])
            vv = t["v_d"].rearrange("(t p) c -> p t c", p=128)

            def load(h):
                s = h % 2
                fw.dma("sp", kT[s][:], t["kT_d"][h], reads=[self.b_kT], writes=[b_k[s]])
                fw.dma("sp", qT[s][:], t["qT_d"][h], reads=[self.b_qT], writes=[b_q[s]])
                fw.dma("sp", V[s][:], vv[:, :, 128 * h:128 * (h + 1)], reads=[self.b_v], writes=[b_V[s]])

            load(0)
            cnt = 0
            for h in range(4):
                s = h % 2
                if h + 1 < 4:
                    load(h + 1)
                for qc in range(4):
                    qs = slice(512 * qc, 512 * (qc + 1))

                    def qk(kb, si):
                        for m in range(2):
                            fw.op("pe", lambda e, m=m, kb=kb, si=si: e.matmul(Sp[si][m][:], kT[s][64 * m:64 * (m + 1), 128 * kb:128 * (kb + 1)], qT[s][64 * m:64 * (m + 1), qs],
                                                                         start=True, stop=True),
                                  reads=[b_k[s], b_q[s]], writes=[b_S[si][m]])
                            fw.op("act", lambda e, m=m, si=si: e.activation(out=P[si][m][:], in_=Sp[si][m][:], func=AF.Exp), reads=[b_S[si][m]], writes=[b_P[si][m]])

                    def pv(kb, si):
                        for m in range(2):
                            fw.op("pe", lambda e, m=m, kb=kb, si=si: e.matmul(Op[m][:], V[s][:, kb, :], P[si][m][:], start=(kb == 0), stop=(kb == 31)),
                                  reads=[b_V[s], b_P[si][m]], writes=[b_O[m]])
                            fw.op("pe", lambda e, m=m, kb=kb, si=si: e.matmul(Dp[m][:], self.onesb[:], P[si][m][:], start=(kb == 0), stop=(kb == 31)),
                                  reads=[self.b_const, b_P[si][m]], writes=[b_D[m]])

                    qk(0, cnt % 2)
                    for kb in range(32):
                        si = cnt % 2
                        cnt += 1
                        if kb + 1 < 32:
                            qk(kb + 1, cnt % 2)
                        pv(kb, si)
                    oi = (h * 4 + qc) % 2
                    fw.op("dve", lambda e: e.reciprocal(out=r0[:], in_=Dp[0][:]), reads=[b_D[0]], writes=[b_f])
                    fw.op("dve", lambda e: e.reciprocal(out=r1[:], in_=Dp[1][:]), reads=[b_D[1]], writes=[b_f])
                    fw.op("dve", lambda e: e.tensor_tensor(out=o0[:], in0=Op[0][:], in1=r0[:], op=ALU.mult), reads=[b_O[0], b_f], writes=[b_f])
                    fw.op("dve", lambda e: e.tensor_tensor(out=o1[:], in0=Op[1][:], in1=r1[:], op=ALU.mult), reads=[b_O[1], b_f], writes=[b_f])
                    fw.op("dve", lambda e: e.scalar_tensor_tensor(out=o0[:], in0=o1[:], scalar=lamcol[:, 0:1], in1=o0[:], op0=ALU.mult, op1=ALU.add),
                          reads=[b_f, b_lc], writes=[b_f])
                    fw.op("act", lambda e: e.activation(out=sq[:], in_=o0[:], func=AF.Square), reads=[b_f], writes=[b_f])
                    si = cnt % 2
                    fw.op("pe", lambda e, si=si: e.matmul(Sp[si][0][:], self.onesb[:], sq[:], start=True, stop=True), reads=[b_f, self.b_const], writes=[b_S[si][0]])
                    fw.op("act", lambda e, si=si: e.activation(out=rinv[:], in_=Sp[si][0][:], func=AF.Sqrt, scale=1.0 / 128, bias=self.eps_col[:, 0:1]),
                          reads=[b_S[si][0], self.b_const], writes=[b_f])
                    fw.op("dve", lambda e: e.reciprocal(out=rinv[:], in_=rinv[:]), reads=[b_f], writes=[b_f])
                    fw.op("dve", lambda e: e.tensor_tensor(out=o0[:], in0=o0[:], in1=rinv[:], op=ALU.mult), reads=[b_f], writes=[b_f])
                    fw.op("dve", lambda e, oi=oi: e.tensor_scalar(out=ob[oi][:], in0=o0[:], scalar1=gcol[:, 0:1], scalar2=None, op0=ALU.mult),
                          reads=[b_f, b_lc], writes=[b_ob[oi]])
                    fw.dma("sp", t["oT_d"][h, :, qs], ob[oi][:], reads=[b_ob[oi]], writes=[self.b_oT[h]])
            fw.barrier()

    def phase_na(self):
        nc, fw, t = self.nc, self.fw, self.t
        with ExitStack() as es:
            kT = [self.sb(es, f"na_kT{i}", [128, NCTX], BF16) for i in range(2)]
            qT = [self.sb(es, f"na_qT{i}", [128, TOWN], BF16) for i in range(2)]
            Vp = [[self.sb(es, f"na_V{i}{hh}", [128, 20, 128], BF16) for hh in range(2)] for i in range(2)]
            rm = self.sb(es, "na_rm", [128, 24, 512], BF16)
            bt = self.sb(es, "na_bt", [128, 8, 512], F32)
            mk = [self.sb(es, f"na_mk{hh}", [128, 24, 512], BF16) for hh in range(2)]
            onesp = [self.sb(es, f"na_ones{hh}", [128, 128], BF16) for hh in range(2)]
            P = [self.sb(es, f"na_P{i}", [128, 512], BF16) for i in range(2)]
            rr = self.sb(es, "na_r", [128, 512], F32)
            ob = [self.sb(es, f"na_ob{i}", [128, 512], BF16) for i in range(2)]
            Sp = [self.ps(es, f"na_S{i}", [128, 512]) for i in range(2)]
            Op = [self.ps(es, f"na_O{i}", [128, 512]) for i in range(2)]
            Dp = [self.ps(es, f"na_D{i}", [128, 512]) for i in range(2)]
            b_k = fw.bufs("nak", 2); b_q = fw.bufs("naq", 2); b_V = [fw.bufs(f"naV{i}", 2) for i in range(2)]
            b_rm = fw.buf("narm"); b_bt = fw.buf("nabt"); b_mk = fw.bufs("namk", 2); b_on = fw.buf("naones")
            b_P = fw.bufs("naP", 2); b_S = fw.bufs("naS", 2); b_O = fw.bufs("naO", 2); b_D = fw.bufs("naD", 2)
            b_r = fw.buf("nar"); b_ob = fw.bufs("naob", 2)
            fw.dma("pool", rm[:], t["na_rowmask"].rearrange("n p c -> p n c"), writes=[b_rm])
            for hh in range(2):
                fw.op("dve", lambda e, hh=hh: e.memset(onesp[hh][:], 0.0), writes=[b_on])
                fw.op("dve", lambda e, hh=hh: e.memset(onesp[hh][:, 64 * hh:64 * (hh + 1)], 1.0), writes=[b_on])
            vv = t["v_d"].rearrange("(t p) c -> p t c", p=128)
            cnt = 0
            for pr in range(2):
                s = pr % 2
                fw.dma("sp", kT[s][:], t["kT_d"][4 + pr], reads=[self.b_kT], writes=[b_k[s]])
                fw.dma("sp", qT[s][:], t["qT_d"][4 + pr], reads=[self.b_qT], writes=[b_q[s]])
                for hh in range(2):
                    h = 2 * pr + hh
                    fw.op("pool", lambda e, hh=hh: e.memset(Vp[s][hh][:], 0.0), writes=[b_V[s][hh]])
                    fw.dma("sp", Vp[s][hh][:, :, 64 * hh:64 * (hh + 1)], vv[:, 6:26, 512 + 64 * h:512 + 64 * (h + 1)], reads=[self.b_v], writes=[b_V[s][hh]])
                    fw.op("pool", lambda e: e.memset(bt[:], 0.0), writes=[b_bt])
                    for i in range(8):
                        for kr in range(2):
                            rk = -4 + 2 * i + kr
                            lo, hi = max(0, rk - 7), min(7, rk + 7)
                            n = hi - lo + 1
                            j0 = 7 - rk + lo
                            fw.dma("sp", bt[64 * kr:64 * (kr + 1), i, 64 * lo:64 * (hi + 1)].rearrange("p (n c) -> p n c", c=64),
                                   t["na_tab"][h, j0:j0 + n].rearrange("n k c -> k n c"), writes=[b_bt])
                    for q in range(24):
                        eng = "dve" if q % 2 == 0 else "pool"
                        fw.op(eng, lambda e, q=q, hh=hh: e.tensor_tensor(out=mk[hh][:, q, :], in0=bt[:, q % 8, :], in1=rm[:, q, :], op=ALU.add),
                              reads=[b_bt, b_rm], writes=[b_mk[hh]])
                for qc in range(4):
                    qs = slice(512 * qc, 512 * (qc + 1))
                    sset = 0 if qc == 0 else (2 if qc == 3 else 1)
                    oi = qc % 2
                    units = [(hh, i) for hh in range(2) for i in range(8)]

                    def sc(u, si):
                        hh, i = units[u]
                        tl = 6 + 4 * qc + i
                        fw.op("pe", lambda e, hh=hh, tl=tl, si=si: e.matmul(Sp[si][:], kT[s][64 * hh:64 * (hh + 1), 128 * tl:128 * (tl + 1)], qT[s][64 * hh:64 * (hh + 1), qs],
                                                                     start=True, stop=False),
                              reads=[b_k[s], b_q[s]], writes=[b_S[si]])
                        fw.op("pe", lambda e, hh=hh, i=i, si=si: e.matmul(Sp[si][:], self.identb[:], mk[hh][:, sset * 8 + i, :], start=False, stop=True),
                              reads=[self.b_const, b_mk[hh]], writes=[b_S[si]])
                        fw.op("act", lambda e, si=si: e.activation(out=P[si][:], in_=Sp[si][:], func=AF.Exp), reads=[b_S[si]], writes=[b_P[si]])

                    def pv(u, si):
                        hh, i = units[u]
                        tl = 4 * qc + i
                        fw.op("pe", lambda e, hh=hh, tl=tl, si=si, u=u: e.matmul(Op[oi][:], Vp[s][hh][:, tl, :], P[si][:], start=(u == 0), stop=(u == 15)),
                              reads=[b_V[s][hh], b_P[si]], writes=[b_O[oi]])
                        fw.op("pe", lambda e, hh=hh, si=si, u=u: e.matmul(Dp[oi][:], onesp[hh][:], P[si][:], start=(u == 0), stop=(u == 15)),
                              reads=[b_on, b_P[si]], writes=[b_D[oi]])

                    sc(0, cnt % 2)
                    for u in range(16):
                        si = cnt % 2
                        cnt += 1
                        if u + 1 < 16:
                            sc(u + 1, cnt % 2)
                        pv(u, si)
                    fw.op("dve", lambda e, oi=oi: e.reciprocal(out=rr[:], in_=Dp[oi][:]), reads=[b_D[oi]], writes=[b_r])
                    fw.op("dve", lambda e, oi=oi: e.tensor_tensor(out=ob[oi][:], in0=Op[oi][:], in1=rr[:], op=ALU.mult), reads=[b_O[oi], b_r], writes=[b_ob[oi]])
                    fw.dma("sp", t["oT_d"][4 + pr, :, qs], ob[oi][:], reads=[b_ob[oi]], writes=[self.b_oT[4 + pr]])
            fw.barrier()

    def phase_dil(self):
        nc, fw, t = self.nc, self.fw, self.t
        with ExitStack() as es:
            kT = [self.sb(es, f"dl_kT{i}", [128, NCTX], BF16) for i in range(2)]
            qT = [self.sb(es, f"dl_qT{i}", [128, TOWN], BF16) for i in range(2)]
            Vg = [[self.sb(es, f"dl_V{i}{hh}", [128, 32, 128], BF16) for hh in range(2)] for i in range(2)]
            dm = self.sb(es, "dl_mask", [128, 7, 128], BF16)
            Og = [self.sb(es, f"dl_Og{g}", [128, TOWN], F32) for g in range(3)]
            Dg = [self.sb(es, f"dl_Dg{g}", [128, TOWN], F32) for g in range(3)]
            onesp = [self.sb(es, f"dl_ones{hh}", [128, 128], BF16) for hh in range(2)]
            P = [self.sb(es, f"dl_P{i}", [128, 3, 128], BF16) for i in range(2)]
            ob = self.sb(es, "dl_ob", [128, TOWN], BF16)
            Sp = [self.ps(es, f"dl_S{i}", [128, 3, 128]) for i in range(2)]
            Op = [self.ps(es, f"dl_O{i}", [128, 128]) for i in range(2)]
            Dp = [self.ps(es, f"dl_D{i}", [128, 128]) for i in range(2)]
            b_k = fw.bufs("dlk", 2); b_q = fw.bufs("dlq", 2); b_V = [fw.bufs(f"dlV{i}", 2) for i in range(2)]
            b_dm = fw.buf("dlm"); b_on = fw.buf("dlones"); b_Og = fw.bufs("dlOg", 3); b_Dg = fw.bufs("dlDg", 3)
            b_P = fw.bufs("dlP", 2); b_S = fw.bufs("dlS", 2); b_O = fw.bufs("dlO", 2); b_D = fw.bufs("dlD", 2); b_ob = fw.buf("dlob")
            fw.dma("pool", dm[:], t["dil_mask"].rearrange("n p c -> p n c"), writes=[b_dm])
            for hh in range(2):
                fw.op("dve", lambda e, hh=hh: e.memset(onesp[hh][:], 0.0), writes=[b_on])
                fw.op("dve", lambda e, hh=hh: e.memset(onesp[hh][:, 64 * hh:64 * (hh + 1)], 1.0), writes=[b_on])
            cnt = 0
            ucnt = 0
            gi = 0
            for pr in range(2):
                for g, d in enumerate((1, 4, 16)):
                    s = gi % 2
                    gi += 1
                    ch = 6 + 2 * g + pr
                    nub = 32 // d
                    fw.dma("sp", kT[s][:], t["kT_d"][ch], reads=[self.b_kT], writes=[b_k[s]])
                    fw.dma("sp", qT[s][:], t["qT_d"][ch], reads=[self.b_qT], writes=[b_q[s]])
                    for hh in range(2):
                        col0 = 768 + g * 256 + (2 * pr + hh) * 64
                        fw.op("pool", lambda e, hh=hh: e.memset(Vg[s][hh][:], 0.0), writes=[b_V[s][hh]])
                        for dd in range(d):
                            src = t["v_d"].rearrange("(ub p dd) c -> p dd ub c", p=128, dd=d)[:, dd, :, col0:col0 + 64]
                            fw.dma("sp", Vg[s][hh][:, dd * nub:(dd + 1) * nub, 64 * hh:64 * (hh + 1)], src, reads=[self.b_v], writes=[b_V[s][hh]])
                    kv = kT[s][:, :].rearrange("p (s dd) -> p s dd", dd=d)
                    qv = qT[s][:, :].rearrange("p (s dd) -> p s dd", dd=d)
                    n_mb = 16 // d
                    subs = []
                    for r in range(d):
                        for mb in range(n_mb):
                            if d == 16:
                                blks = [(0, 5), (1, 6)]
                            else:
                                ub0 = (1024 // d) // 128 + mb - 1
                                blks = [(ub0, 0 if mb == 0 else 1), (ub0 + 1, 2), (ub0 + 2, 4 if mb == n_mb - 1 else 3)]
                            for hh in range(2):
                                subs.append((r, mb, hh, blks))

                    def sc(u, si):
                        r, mb, hh, blks = subs[u]
                        for bi, (ub, mi) in enumerate(blks):
                            fw.op("pe", lambda e, hh=hh, ub=ub, r=r, mb=mb, bi=bi, si=si: e.matmul(
                                Sp[si][:, bi, :], kv[64 * hh:64 * (hh + 1), 128 * ub:128 * (ub + 1), r], qv[64 * hh:64 * (hh + 1), 128 * mb:128 * (mb + 1), r],
                                start=True, stop=False), reads=[b_k[s], b_q[s]], writes=[b_S[si]])
                            fw.op("pe", lambda e, mi=mi, bi=bi, si=si: e.matmul(Sp[si][:, bi, :], self.identb[:], dm[:, mi, :], start=False, stop=True),
                                  reads=[self.b_const, b_dm], writes=[b_S[si]])
                        nb = len(blks)
                        fw.op("act", lambda e, si=si, nb=nb: e.activation(out=P[si][:, 0:nb, :], in_=Sp[si][:, 0:nb, :], func=AF.Exp), reads=[b_S[si]], writes=[b_P[si]])

                    def pv(u, si, oi):
                        r, mb, hh, blks = subs[u]
                        nb = len(blks)
                        for bi, (ub, mi) in enumerate(blks):
                            first = (hh == 0 and bi == 0); last = (hh == 1 and bi == nb - 1)
                            fw.op("pe", lambda e, hh=hh, ub=ub, r=r, bi=bi, si=si, first=first, last=last: e.matmul(
                                Op[oi][:], Vg[s][hh][:, r * nub + ub, :], P[si][:, bi, :], start=first, stop=last),
                                reads=[b_V[s][hh], b_P[si]], writes=[b_O[oi]])
                            fw.op("pe", lambda e, hh=hh, bi=bi, si=si, first=first, last=last: e.matmul(
                                Dp[oi][:], onesp[hh][:], P[si][:, bi, :], start=first, stop=last),
                                reads=[b_on, b_P[si]], writes=[b_D[oi]])
                        if hh == 1:
                            ov = Og[g][:, :].rearrange("p (s dd) -> p s dd", dd=d)[:, 128 * mb:128 * (mb + 1), r]
                            dv = Dg[g][:, :].rearrange("p (s dd) -> p s dd", dd=d)[:, 128 * mb:128 * (mb + 1), r]
                            fw.op("act", lambda e, ov=ov, oi=oi: e.activation(out=ov, in_=Op[oi][:], func=AF.Copy), reads=[b_O[oi]], writes=[b_Og[g]])
                            fw.op("dve", lambda e, dv=dv, oi=oi: e.tensor_copy(out=dv, in_=Dp[oi][:]), reads=[b_D[oi]], writes=[b_Dg[g]])

                    sc(0, cnt % 2)
                    for u in range(len(subs)):
                        si = cnt % 2
                        cnt += 1
                        if u + 1 < len(subs):
                            sc(u + 1, cnt % 2)
                        pv(u, si, (ucnt // 2) % 2)
                        ucnt += 1
                fw.op("pool", lambda e: e.tensor_tensor(out=Dg[0][:], in0=Dg[0][:], in1=Dg[1][:], op=ALU.add), reads=[b_Dg[0], b_Dg[1]], writes=[b_Dg[0]])
                fw.op("pool", lambda e: e.tensor_tensor(out=Dg[0][:], in0=Dg[0][:], in1=Dg[2][:], op=ALU.add), reads=[b_Dg[0], b_Dg[2]], writes=[b_Dg[0]])
                fw.op("pool", lambda e: e.tensor_tensor(out=Og[0][:], in0=Og[0][:], in1=Og[1][:], op=ALU.add), reads=[b_Og[0], b_Og[1]], writes=[b_Og[0]])
                fw.op("pool", lambda e: e.tensor_tensor(out=Og[0][:], in0=Og[0][:], in1=Og[2][:], op=ALU.add), reads=[b_Og[0], b_Og[2]], writes=[b_Og[0]])
                fw.op("dve", lambda e: e.reciprocal(out=Dg[0][:], in_=Dg[0][:]), reads=[b_Dg[0]], writes=[b_Dg[0]])
                fw.op("dve", lambda e: e.tensor_tensor(out=ob[:], in0=Og[0][:], in1=Dg[0][:], op=ALU.mult), reads=[b_Og[0], b_Dg[0]], writes=[b_ob])
                fw.dma("sp", t["oT_d"][6 + pr], ob[:], reads=[b_ob], writes=[self.b_oT[6 + pr]])
            fw.barrier()

    def phase_proj(self):
        nc, fw, t = self.nc, self.fw, self.t
        self.b_xmid = fw.bufs("x_mid", 16)
        with ExitStack() as es:
            Wa = self.sb(es, "pj_Wa", [128, 4, D], BF16); Wn = self.sb(es, "pj_Wn", [128, 2, D], BF16)
            Wd = self.sb(es, "pj_Wd", [128, 2, D], BF16); Wo = self.sb(es, "pj_Wo", [128, 8, D], BF16)
            oT = [self.sb(es, f"pj_oT{i}", [128, 8, 512], BF16) for i in range(2)]
            gT = [self.sb(es, f"pj_gT{i}", [128, 24, 512], BF16) for i in range(2)]
            m1 = [self.sb(es, f"pj_m1{i}", [128, 512], F32) for i in range(2)]
            m2 = [self.sb(es, f"pj_m2{i}", [128, 512], F32) for i in range(2)]
            m3 = [self.sb(es, f"pj_m3{i}", [128, 512], F32) for i in range(2)]
            mT = [self.sb(es, f"pj_mT{i}", [128, 8, 512], BF16) for i in range(2)]
            xt = [self.sb(es, f"pj_xt{i}", [128, D], F32) for i in range(2)]
            yt = [self.sb(es, f"pj_yt{i}", [128, D], F32) for i in range(2)]
            pa = [self.ps(es, f"pj_pa{i}", [128, 512]) for i in range(2)]
            pn = [self.ps(es, f"pj_pn{i}", [128, 512]) for i in range(2)]
            pd = [self.ps(es, f"pj_pd{i}", [128, 512]) for i in range(2)]
            py = [self.ps(es, f"pj_py{i}", [128, 512]) for i in range(2)]
            b_W = fw.buf("pjW"); b_oT = fw.bufs("pjoT", 2); b_gT = fw.bufs("pjgT", 2)
            b_m1 = fw.bufs("pjm1", 2); b_m2 = fw.bufs("pjm2", 2); b_m3 = fw.bufs("pjm3", 2); b_mT = fw.bufs("pjmT", 2)
            b_xt = fw.bufs("pjxt", 2); b_yt = fw.bufs("pjyt", 2)
            b_pa = fw.bufs("pjpa", 2); b_pn = fw.bufs("pjpn", 2); b_pd = fw.bufs("pjpd", 2); b_py = fw.bufs("pjpy", 2)
            fw.dma("pool", Wa[:], t["w_proj_a"].rearrange("(j p) n -> p j n", p=128), writes=[b_W])
            fw.dma("pool", Wn[:], t["w_proj_n"].rearrange("(j p) n -> p j n", p=128), writes=[b_W])
            fw.dma("pool", Wd[:], t["w_proj_d"].rearrange("(j p) n -> p j n", p=128), writes=[b_W])
            fw.dma("pool", Wo[:], t["w_out"].rearrange("(j p) n -> p j n", p=128), writes=[b_W])

            def load(qc):
                s = qc % 2
                fw.dma("sp", oT[s][:], t["oT_d"][:, :, 512 * qc:512 * (qc + 1)].rearrange("j p n -> p j n"), reads=self.b_oT, writes=[b_oT[s]])
                fw.dma("sp", gT[s][:], t["gT_d"][:, :, 512 * qc:512 * (qc + 1)].rearrange("j p n -> p j n"), reads=[self.b_gT], writes=[b_gT[s]])

            load(0)
            cnt = 0
            ycnt = 0
            for qc in range(4):
                s = qc % 2
                if qc + 1 < 4:
                    load(qc + 1)
                for c in range(8):
                    i = cnt % 2
                    cnt += 1
                    cs = slice(128 * c, 128 * (c + 1))
                    for j in range(4):
                        fw.op("pe", lambda e, j=j, i=i, cs=cs: e.matmul(pa[i][:], Wa[:, j, cs], oT[s][:, j, :], start=(j == 0), stop=(j == 3)),
                              reads=[b_W, b_oT[s]], writes=[b_pa[i]])
                    for j in range(2):
                        fw.op("pe", lambda e, j=j, i=i, cs=cs: e.matmul(pn[i][:], Wn[:, j, cs], oT[s][:, 4 + j, :], start=(j == 0), stop=(j == 1)),
                              reads=[b_W, b_oT[s]], writes=[b_pn[i]])
                    for j in range(2):
                        fw.op("pe", lambda e, j=j, i=i, cs=cs: e.matmul(pd[i][:], Wd[:, j, cs], oT[s][:, 6 + j, :], start=(j == 0), stop=(j == 1)),
                              reads=[b_W, b_oT[s]], writes=[b_pd[i]])
                    fw.op("dve", lambda e, i=i, c=c: e.tensor_tensor(out=m1[i][:], in0=pa[i][:], in1=gT[s][:, c, :], op=ALU.mult), reads=[b_pa[i], b_gT[s]], writes=[b_m1[i]])
                    fw.op("dve", lambda e, i=i, c=c: e.tensor_tensor(out=m2[i][:], in0=pn[i][:], in1=gT[s][:, 8 + c, :], op=ALU.mult), reads=[b_pn[i], b_gT[s]], writes=[b_m2[i]])
                    fw.op("dve", lambda e, i=i, c=c: e.tensor_tensor(out=m3[i][:], in0=pd[i][:], in1=gT[s][:, 16 + c, :], op=ALU.mult), reads=[b_pd[i], b_gT[s]], writes=[b_m3[i]])
                    fw.op("pool", lambda e, i=i: e.tensor_tensor(out=m1[i][:], in0=m1[i][:], in1=m2[i][:], op=ALU.add), reads=[b_m1[i], b_m2[i]], writes=[b_m1[i]])
                    fw.op("pool", lambda e, i=i, c=c: e.tensor_tensor(out=mT[s][:, c, :], in0=m1[i][:], in1=m3[i][:], op=ALU.add), reads=[b_m1[i], b_m3[i]], writes=[b_mT[s]])
                for tq in range(4):
                    tile = 4 * qc + tq
                    xi = tile % 2
                    fw.dma("sp", xt[xi][:], t["xc"][OWN0 + 128 * tile:OWN0 + 128 * (tile + 1), :], writes=[b_xt[xi]])
                    for half in range(2):
                        yi = ycnt % 2
                        ycnt += 1
                        hs = slice(512 * half, 512 * (half + 1))
                        for c in range(8):
                            fw.op("pe", lambda e, c=c, yi=yi, tq=tq, hs=hs: e.matmul(py[yi][:], mT[s][:, c, 128 * tq:128 * (tq + 1)], Wo[:, c, hs], start=(c == 0), stop=(c == 7)),
                                  reads=[b_mT[s], b_W], writes=[b_py[yi]])
                        fw.op("dve", lambda e, yi=yi, xi=xi, hs=hs: e.tensor_tensor(out=yt[xi][:, hs], in0=py[yi][:], in1=self.gate_bc[:, hs], op=ALU.mult),
                              reads=[b_py[yi], self.b_mod], writes=[b_yt[xi]])
                    fw.op("pool", lambda e, xi=xi: e.tensor_tensor(out=yt[xi][:], in0=yt[xi][:], in1=xt[xi][:], op=ALU.add), reads=[b_yt[xi], b_xt[xi]], writes=[b_yt[xi]])
                    fw.dma("sp", t["x_mid"][128 * tile:128 * (tile + 1), :], yt[xi][:], reads=[b_yt[xi]], writes=[self.b_xmid[tile]])
            fw.barrier()

    def router_tile(self, i, h32, b_h32):
        fw = self.fw
        R = self.rt
        s = i % 2
        lgp, b_lgp, w, b = R["lgp"][s], R["b_lgp"][s], R["w"][s], R["b_w"][s]
        Wt, b_Wt = R["Wt"], R["b_Wt"]
        for k in range(8):
            fw.op("pe", lambda e, k=k: e.matmul(lgp[:, 0:36], h32[:, k, :], R["Wr"][:, k, :], start=(k == 0), stop=False),
                  reads=[b_h32, R["b_W"]], writes=[b_lgp])
        fw.op("pe", lambda e: e.matmul(lgp[:, 0:36], self.ones32[0:1, :], R["br"][0:1, :], start=False, stop=True),
              reads=[R["b_W"], self.b_const], writes=[b_lgp])
        D_ = lambda fn, rd=(): fw.op("dve", fn, reads=[b] + list(rd), writes=[b])
        fw.op("dve", lambda e: e.tensor_copy(out=w[:, 0:36], in_=lgp[:, 0:36]), reads=[b_lgp], writes=[b])
        D_(lambda e: e.reduce_max(out=w[:, 36:37], in_=w[:, 0:4], axis=AX.X))
        D_(lambda e: e.tensor_scalar(out=w[:, 40:44], in0=w[:, 0:4], scalar1=w[:, 36:37], scalar2=None, op0=ALU.is_equal))
        D_(lambda e: e.tensor_scalar(out=w[:, 37:38], in0=w[:, 36:37], scalar1=-1.0, scalar2=None, op0=ALU.mult))
        fw.op("act", lambda e: e.activation(out=w[:, 153:157], in_=w[:, 0:4], func=AF.Exp, bias=w[:, 37:38], accum_out=w[:, 38:39]), reads=[b], writes=[b])
        D_(lambda e: e.reciprocal(out=w[:, 39:40], in_=w[:, 38:39]))
        D_(lambda e: e.tensor_scalar(out=w[:, 44:48], in0=w[:, 40:44], scalar1=-1.0, scalar2=1e30, op0=ALU.add, op1=ALU.mult))
        for g in range(4):
            D_(lambda e, g=g: e.tensor_scalar(out=w[:, 48 + 8 * g:56 + 8 * g], in0=w[:, 4 + 8 * g:12 + 8 * g], scalar1=w[:, 44 + g:45 + g], scalar2=None, op0=ALU.add))
        D_(lambda e: e.reduce_max(out=w[:, 80:81], in_=w[:, 48:80], axis=AX.X))
        D_(lambda e: e.tensor_scalar(out=w[:, 84:116], in0=w[:, 48:80], scalar1=w[:, 80:81], scalar2=None, op0=ALU.is_equal))
        D_(lambda e: e.scalar_tensor_tensor(out=w[:, 116:148], in0=w[:, 84:116], scalar=-1e30, in1=w[:, 48:80], op0=ALU.mult, op1=ALU.add))
        D_(lambda e: e.reduce_max(out=w[:, 148:149], in_=w[:, 116:148], axis=AX.X))
        D_(lambda e: e.tensor_scalar(out=w[:, 157:189], in0=w[:, 116:148], scalar1=w[:, 148:149], scalar2=None, op0=ALU.is_equal))
        D_(lambda e: e.tensor_tensor(out=w[:, 149:150], in0=w[:, 80:81], in1=w[:, 148:149], op=ALU.subtract))
        fw.op("act", lambda e: e.activation(out=w[:, 150:151], in_=w[:, 149:150], func=AF.Sigmoid), reads=[b], writes=[b])
        D_(lambda e: e.tensor_tensor(out=w[:, 151:152], in0=w[:, 150:151], in1=w[:, 39:40], op=ALU.mult))
        D_(lambda e: e.tensor_tensor(out=w[:, 152:153], in0=w[:, 39:40], in1=w[:, 151:152], op=ALU.subtract))
        fw.op("dve", lambda e: e.tensor_scalar(out=Wt[:, i, :], in0=w[:, 84:116], scalar1=w[:, 151:152], scalar2=None, op0=ALU.mult), reads=[b], writes=[b_Wt[i]])
        fw.op("dve", lambda e: e.scalar_tensor_tensor(out=Wt[:, i, :], in0=w[:, 157:189], scalar=w[:, 152:153], in1=Wt[:, i, :], op0=ALU.mult, op1=ALU.add),
              reads=[b, b_Wt[i]], writes=[b_Wt[i]])

    def phase_ffn(self):
        nc, fw, t = self.nc, self.fw, self.t
        NT = TOWN // 128
        with ExitStack() as hs:
            self.hT = self.sb(hs, "hT_f", [128, 8, TOWN], BF16)
            self.b_hT = fw.bufs("hTf", NT)
            Wt = self.sb(hs, "Wt", [128, NT, 32], F32)
            acc = self.sb(hs, "acc", [128, NT, D], F32)
            with ExitStack() as es:
                R = self.rt = {}
                R["Wt"] = Wt; R["b_Wt"] = fw.bufs("Wt", NT)
                R["Wr"] = self.sb(es, "Wr", [128, 8, 36], F32); R["br"] = self.sb(es, "br", [1, 36], F32); R["b_W"] = fw.buf("Wr")
                R["lgp"] = [self.ps(es, f"lgp{i}", [128, 64]) for i in range(2)]; R["b_lgp"] = fw.bufs("lgp", 2)
                R["w"] = [self.sb(es, f"rw{i}", [128, 192], F32) for i in range(2)]; R["b_w"] = fw.bufs("rw", 2)
                fw.dma("sp", R["Wr"][:], t["w_router"].rearrange("(k p) n -> p k n", p=128), writes=[R["b_W"]])
                fw.dma("sp", R["br"][:], t["b_router"][:, :], writes=[R["b_W"]])
                self.phase_norm(t["x_mid"], NT, self.gs_f, self.sh_f, router=True)
            if self.debug:
                fw.dma("sp", t["dbg_Wt"].rearrange("(n p) e -> p n e", p=128), Wt[:], reads=R["b_Wt"])
            if self.stop_after == "router":
                return
            b_Wt = R["b_Wt"]
            with ExitStack() as es:
                w1s = [self.sb(es, f"w1s{i}", [128, 8, HID], BF16) for i in range(2)]
                w3s = [self.sb(es, f"w3s{i}", [128, 8, HID], BF16) for i in range(2)]
                w2s = [self.sb(es, f"w2s{i}", [128, 4, D], BF16) for i in range(2)]
                s1 = [self.sb(es, f"s1_{i}", [128, 512], BF16) for i in range(2)]
                gTe = [self.sb(es, f"gTe{i}", [128, 4, 512], BF16) for i in range(2)]
                h1 = [self.ps(es, f"h1p{i}", [128, 512]) for i in range(2)]
                h3 = [self.ps(es, f"h3p{i}", [128, 512]) for i in range(2)]
                yp = [self.ps(es, f"yp{i}", [128, 512]) for i in range(2)]
                b_w = fw.bufs("ew", 2); b_s1 = fw.bufs("s1", 2); b_g = fw.bufs("gTe", 2)
                b_h1 = fw.bufs("h1", 2); b_h3 = fw.bufs("h3", 2); b_y = fw.bufs("yp", 2)
                b_acc = [fw.bufs(f"acc{i}_", 2) for i in range(NT)]

                def load(e_):
                    s = e_ % 2
                    fw.dma("pool", w1s[s][:], t["w1"][e_].rearrange("(k p) n -> p k n", p=128), writes=[b_w[s]])
                    fw.dma("pool", w3s[s][:], t["w3"][e_].rearrange("(k p) n -> p k n", p=128), writes=[b_w[s]])
                    fw.dma("pool", w2s[s][:], t["w2"][e_].rearrange("(k p) n -> p k n", p=128), writes=[b_w[s]])

                load(0)
                hcnt = 0; gcnt = 0; ycnt = 0
                for ex in range(self.n_exp):
                    s = ex % 2
                    if ex + 1 < self.n_exp:
                        load(ex + 1)
                    for tc in range(4):
                        gi = gcnt % 2; gcnt += 1
                        ts_ = slice(512 * tc, 512 * (tc + 1))
                        for hc in range(4):
                            hi = hcnt % 2; hcnt += 1
                            hsl = slice(128 * hc, 128 * (hc + 1))
                            for k in range(8):
                                fw.op("pe", lambda e, k=k, hi=hi, hsl=hsl, ts_=ts_, s=s: e.matmul(h1[hi][:], w1s[s][:, k, hsl], self.hT[:, k, ts_], start=(k == 0), stop=(k == 7)),
                                      reads=[b_w[s]] + self.b_hT[4 * tc:4 * tc + 4], writes=[b_h1[hi]])
                            for k in range(8):
                                fw.op("pe", lambda e, k=k, hi=hi, hsl=hsl, ts_=ts_, s=s: e.matmul(h3[hi][:], w3s[s][:, k, hsl], self.hT[:, k, ts_], start=(k == 0), stop=(k == 7)),
                                      reads=[b_w[s]] + self.b_hT[4 * tc:4 * tc + 4], writes=[b_h3[hi]])
                            fw.op("act", lambda e, hi=hi: e.activation(out=s1[hi][:], in_=h1[hi][:], func=AF.Silu), reads=[b_h1[hi]], writes=[b_s1[hi]])
                            fw.op("dve", lambda e, hi=hi, gi=gi, hc=hc: e.tensor_tensor(out=gTe[gi][:, hc, :], in0=s1[hi][:], in1=h3[hi][:], op=ALU.mult),
                                  reads=[b_s1[hi], b_h3[hi]], writes=[b_g[gi]])
                        for tq in range(4):
                            tile = 4 * tc + tq
                            for half in range(2):
                                yi = ycnt % 2; ycnt += 1
                                hs_ = slice(512 * half, 512 * (half + 1))
                                for hc in range(4):
                                    fw.op("pe", lambda e, hc=hc, yi=yi, gi=gi, tq=tq, hs_=hs_, s=s: e.matmul(yp[yi][:], gTe[gi][:, hc, 128 * tq:128 * (tq + 1)], w2s[s][:, hc, hs_],
                                                                                             start=(hc == 0), stop=(hc == 3)),
                                          reads=[b_g[gi], b_w[s]], writes=[b_y[yi]])
                                if ex == 0:
                                    fw.op("dve", lambda e, yi=yi, tile=tile, hs_=hs_, ex=ex: e.tensor_scalar(out=acc[:, tile, hs_], in0=yp[yi][:], scalar1=Wt[:, tile, ex:ex + 1],
                                                                                                 scalar2=None, op0=ALU.mult),
                                          reads=[b_y[yi], b_Wt[tile]], writes=[b_acc[tile][half]])
                                else:
                                    fw.op("dve", lambda e, yi=yi, tile=tile, hs_=hs_, ex=ex: e.scalar_tensor_tensor(out=acc[:, tile, hs_], in0=yp[yi][:], scalar=Wt[:, tile, ex:ex + 1],
                                                                                                        in1=acc[:, tile, hs_], op0=ALU.mult, op1=ALU.add),
                                          reads=[b_y[yi], b_Wt[tile], b_acc[tile][half]], writes=[b_acc[tile][half]])
                fw.barrier()
            with ExitStack() as es:
                gfr = self.sb(es, "gfin_row", [1, D], F32); gfb = self.sb(es, "gfin_bc", [128, D], F32)
                xm = [self.sb(es, f"fx{i}", [128, D], F32) for i in range(2)]
                xo = [self.sb(es, f"fo{i}", [128, D], F32) for i in range(2)]
                xn = [self.sb(es, f"fn{i}", [128, D], F32) for i in range(2)]
                junk = self.sb(es, "fjunk", [128, D], BF16)
                st = [self.sb(es, f"fst{i}", [128, 4], F32) for i in range(2)]
                pg = [self.ps(es, f"fpg{i}", [128, 512]) for i in range(2)]
                b_g = fw.buf("gf"); b_pg = fw.bufs("fpg", 2); b_xm = fw.bufs("fxm", 2); b_xo = fw.bufs("fxo", 2); b_xn = fw.bufs("fxn", 2)
                b_st = fw.bufs("fst", 2); b_j = fw.buf("fj")
                fw.dma("sp", gfr[:], t["g_fin"][:, :], writes=[b_g])
                for half in range(2):
                    hs_ = slice(512 * half, 512 * (half + 1))
                    fw.op("pe", lambda e, half=half, hs_=hs_: e.matmul(pg[half][:], self.ones32[0:1, :], gfr[0:1, hs_], start=True, stop=True), reads=[b_g, self.b_const], writes=[b_pg[half]])
                    fw.op("act", lambda e, half=half, hs_=hs_: e.activation(out=gfb[:, hs_], in_=pg[half][:], func=AF.Copy), reads=[b_pg[half]], writes=[b_g])
                for tile in range(NT):
                    i = tile % 2
                    rows = slice(128 * tile, 128 * (tile + 1))
                    fw.dma("sp", xm[i][:], t["x_mid"][rows, :], writes=[b_xm[i]])
                    fw.op("dve", lambda e, i=i, tile=tile: e.tensor_tensor(out=xo[i][:], in0=acc[:, tile, :], in1=self.gate_bc[:, D:2 * D], op=ALU.mult),
                          reads=b_acc[tile] + [self.b_mod], writes=[b_xo[i]])
                    fw.op("pool", lambda e, i=i: e.tensor_tensor(out=xo[i][:], in0=xo[i][:], in1=xm[i][:], op=ALU.add), reads=[b_xo[i], b_xm[i]], writes=[b_xo[i]])
                    fw.dma("sp", t["x_out"][rows, :], xo[i][:], reads=[b_xo[i]])
                    fw.op("act", lambda e, i=i: e.activation(out=junk[:], in_=xo[i][:], func=AF.Square, accum_out=st[i][:, 0:1]), reads=[b_xo[i]], writes=[b_j, b_st[i]])
                    fw.op("act", lambda e, i=i: e.activation(out=st[i][:, 1:2], in_=st[i][:, 0:1], func=AF.Sqrt, scale=1.0 / D, bias=self.eps_col[:, 0:1]),
                          reads=[b_st[i], self.b_const], writes=[b_st[i]])
                    fw.op("dve", lambda e, i=i: e.reciprocal(out=st[i][:, 2:3], in_=st[i][:, 1:2]), reads=[b_st[i]], writes=[b_st[i]])
                    fw.op("dve", lambda e, i=i: e.scalar_tensor_tensor(out=xn[i][:], in0=xo[i][:], scalar=st[i][:, 2:3], in1=gfb[:], op0=ALU.mult, op1=ALU.mult),
                          reads=[b_xo[i], b_st[i], b_g], writes=[b_xn[i]])
                    fw.dma("sp", t["xn_out"][rows, :], xn[i][:], reads=[b_xn[i]])
                fw.barrier()


HEAD = 64; S = 4096; D = 1024

def rot_tables():
    inv = 1.0 / (10000.0 ** (np.arange(0, 64, 2, dtype=np.float32) / 64))
    ang = np.arange(S, dtype=np.float32)[:, None] * inv[None, :]
    ang = np.concatenate([ang, ang], axis=-1)
    return np.cos(ang).astype(np.float32), np.sin(ang).astype(np.float32)

def consts():
    ident = np.eye(128, dtype=np.float32)
    rotm = np.zeros((128, 128), np.float32)
    for m in range(128):
        if (m % 64) < 32: rotm[m + 32, m] = -1.0
        else: rotm[m - 32, m] = 1.0
    return ident, rotm

def na_table(rpb):
    cq = np.arange(64)
    c0 = np.clip(cq - 8, 0, 48)
    kc = np.arange(64)
    inwin = (kc[:, None] >= c0[None, :]) & (kc[:, None] < c0[None, :] + 16)
    bc = np.clip(kc[:, None] - cq[None, :] + 15, 0, 30)
    T = rpb[:, :, bc]
    T = np.where(inwin[None, None], T, np.float32(-30000.0)).astype(np.float32)
    return np.ascontiguousarray(T[:, ::-1])

def na_rowmask(hf):
    out = np.full((3, 8, 128, 512), -30000.0, np.float32)
    for s, qc in ((0, 0), (1, 1), (2, 3)):
        for i in range(8):
            for kr in range(2):
                rk_rel = 8 * qc - 4 + 2 * i + kr
                rk = rk_rel + 32 * hf
                for rq_l in range(8):
                    rq = 8 * qc + rq_l + 32 * hf
                    k0 = min(max(rq - 4, 0), 56)
                    if 0 <= rk < 64 and k0 <= rk < k0 + 8:
                        out[s, i, 64 * kr:64 * kr + 64, 64 * rq_l:64 * rq_l + 64] = 0.0
    return out.reshape(24, 128, 512)

def dil_masks(hf):
    p = np.arange(128)[:, None]; i = np.arange(128)[None, :]
    NEG = np.float32(-30000.0)
    b0 = np.where(p - i >= 64, 0.0, NEG).astype(np.float32)
    b1 = np.where(np.abs(p - i) <= 64, 0.0, NEG).astype(np.float32)
    b2 = np.where(p - i <= -64, 0.0, NEG).astype(np.float32)
    allm = np.full((128, 128), NEG, np.float32)
    b0_first = allm if hf == 0 else b0
    b2_last = b2 if hf == 0 else allm
    m0 = np.where(p >= i, 0.0, NEG).astype(np.float32)
    m1 = np.where(p <= i, 0.0, NEG).astype(np.float32)
    if hf == 0: m0 = np.where(p < 64, NEG, m0).astype(np.float32)
    else: m1 = np.where(p >= 64, NEG, m1).astype(np.float32)
    return np.stack([b0_first, b0, b1, b2, b2_last, m0, m1])

def core_inputs(inp, l, b, hf, x_cur, n_exp=32):
    f = np.float32
    shift = hf * 2048 - 1024
    xc = np.roll(x_cur[b], -shift, axis=0)
    cos, sin = rot_tables()
    cosc = np.roll(cos, -shift, axis=0); sinc = np.roll(sin, -shift, axis=0)
    cosk = np.ascontiguousarray(np.concatenate([cosc.T, cosc.T], 0)); sink = np.ascontiguousarray(np.concatenate([sinc.T, sinc.T], 0))
    cosq = np.ascontiguousarray(cosk[:, 1024:3072] * f(0.125)); sinq = np.ascontiguousarray(sink[:, 1024:3072] * f(0.125))
    ident, rotm = consts()
    import math
    lam_init = 0.8 - 0.6 * math.exp(-0.3 * l)
    d = {
        "xc": np.ascontiguousarray(xc), "c_col": np.ascontiguousarray(inp["c"][b].reshape(8, 128).T),
        "ada_w": inp["ada_w"][l], "ada_b": inp["ada_b"][l].reshape(1, -1),
        "ada_b_col": np.ascontiguousarray(inp["ada_b"][l].reshape(48, 128).T),
        "g_mix_col": np.ascontiguousarray(inp["mix_norm_g"][l].reshape(8, 128).T),
        "g_ffn_col": np.ascontiguousarray(inp["ffn_norm_g"][l].reshape(8, 128).T),
        "w_in": inp["w_in"][l], "da_lambda": inp["da_lambda"][l].reshape(1, 256),
        "subln_col": inp["da_subln_g"][l].reshape(128, 1), "lam_init": np.array([[lam_init, 1.0 - lam_init]], f),
        "na_tab": na_table(inp["na_rpb"][l]), "na_rowmask": na_rowmask(hf), "dil_mask": dil_masks(hf),
        "w_proj_a": inp["w_proj_a"][l], "w_proj_n": inp["w_proj_n"][l], "w_proj_d": inp["w_proj_d"][l], "w_out": inp["w_out"][l],
        "w_router": np.ascontiguousarray(np.concatenate([inp["router_group_w"][l], inp["router_expert_w"][l]], 1)),
        "b_router": np.concatenate([inp["router_group_b"][l], inp["router_expert_b"][l]]).reshape(1, 36),
        "w1": inp["expert_w1"][l][:n_exp], "w3": inp["expert_w3"][l][:n_exp], "w2": inp["expert_w2"][l][:n_exp],
        "g_fin": inp["final_norm_g"].reshape(1, -1),
        "cosq": cosq, "sinq": sinq, "cosk": cosk, "sink": sink, "ident": ident, "rotm": rotm,
    }
    return {k: np.ascontiguousarray(v, dtype=f) for k, v in d.items()}


_PROG_CACHE = {}


def _get_program():
    if "nc" not in _PROG_CACHE:
        nc0 = bass.Bass("TRN2", target_bir_lowering=False)
        fw0 = FW(nc0)
        Prog(nc0, fw0, False, None, NEXP).build()
        nc = bass.Bass("TRN2", target_bir_lowering=False)
        fw = FW(nc, marks=fw0.marks)
        Prog(nc, fw, False, None, NEXP).build()
        _PROG_CACHE["nc"] = nc
    return _PROG_CACHE["nc"]


def kernel(**inputs):
    inp = {k: np.asarray(v) for k, v in inputs.items()}
    x_cur = np.ascontiguousarray(inp["x"], dtype=np.float32)
    nc = _get_program()
    cores = [(b, hf) for b in range(4) for hf in range(2)]
    out = None
    for l in range(4):
        in_maps = [core_inputs(inp, l, b, hf, x_cur) for (b, hf) in cores]
        res = run_bass_kernel_spmd(nc, in_maps, core_ids=list(range(8)))
        key = "xn_out" if l == 3 else "x_out"
        x_new = np.empty_like(x_cur)
        for ci, (b, hf) in enumerate(cores):
            x_new[b, 2048 * hf:2048 * (hf + 1)] = np.asarray(res.results[ci][key], dtype=np.float32)
        x_cur = x_new
        out = x_new
    return out
```
